# Optimizing a Trainium2 kernel written in Bass

```python
import math
import jax, jax.numpy as jnp
from jax import lax
import numpy as np

D_MODEL = 2048
BATCH = 4
SEQ = 4096
DEPTH = 2

N_EVEN = (DEPTH + 1) // 2
N_ODD = DEPTH // 2
EPS = 1e-6
NEG_INF = -1e30

ATTN_HEADS = D_MODEL // 256
ATTN_KV_HEADS = ATTN_HEADS // 4
HEAD_DIM = 128
ATTN_WIDTH = ATTN_HEADS * HEAD_DIM
KV_WIDTH = ATTN_KV_HEADS * HEAD_DIM
WINDOW = 128
BLOCK = 128
REL_BUCKETS = 32
REL_MAX_DIST = 128

SSM_WIDTH = D_MODEL - ATTN_WIDTH
SSM_GROUP = 16
SSM_GROUPS = SSM_WIDTH // SSM_GROUP
SSM_STATE = 64

EVEN_IN = ATTN_WIDTH + 2 * KV_WIDTH + SSM_WIDTH

GMLP_WIDTH = D_MODEL
GMLP_CHUNK = 128
GMLP_HEADS = 16
GMLP_HEAD_DIM = GMLP_WIDTH // GMLP_HEADS

N_EXPERTS = 16
EXPERT_FF = D_MODEL
CAPACITY_FACTOR = 2

kernel_name = 'hybrid_swa_s5_gmlp_ecmoe_encoder'


def rms_norm(x, g):
    xf = x.astype(jnp.float32)
    y = xf * lax.rsqrt(jnp.mean(xf * xf, axis=-1, keepdims=True) + EPS)
    return (y * g.astype(jnp.float32)).astype(x.dtype)


def t5_bucket(rel):
    nb = REL_BUCKETS // 2
    max_exact = nb // 2
    base = jnp.where(rel > 0, nb, 0)
    n = jnp.abs(rel)
    nf = jnp.maximum(n, 1).astype(jnp.float32)
    large = max_exact + (jnp.log(nf / max_exact) / math.log(REL_MAX_DIST / max_exact)
                         * (nb - max_exact)).astype(jnp.int32)
    large = jnp.minimum(large, nb - 1)
    return base + jnp.where(n < max_exact, n, large)


def windowed_attention(q, k, v, sink, rel_bias):
    b, s = q.shape[0], q.shape[1]
    nblk = s // BLOCK
    grp = ATTN_HEADS // ATTN_KV_HEADS
    qb = q.reshape(b, nblk, BLOCK, ATTN_KV_HEADS, grp, HEAD_DIM)

    def band(t):
        tp = jnp.pad(t, ((0, 0), (BLOCK, BLOCK), (0, 0), (0, 0)))
        tp = tp.reshape(b, nblk + 2, BLOCK, ATTN_KV_HEADS, HEAD_DIM)
        return jnp.concatenate([tp[:, :-2], tp[:, 1:-1], tp[:, 2:]], axis=2)

    kb, vb = band(k), band(v)
    scores = jnp.einsum('bnqkgd,bnckd->bnkgqc', qb, kb).astype(jnp.float32) * (HEAD_DIM ** -0.5)

    q_off = jnp.arange(BLOCK, dtype=jnp.int32)
    c_off = jnp.arange(3 * BLOCK, dtype=jnp.int32)
    rel = c_off[None, :] - BLOCK - q_off[:, None]
    bias = rel_bias.astype(jnp.float32)[t5_bucket(rel)]
    bias = jnp.transpose(bias, (2, 0, 1)).reshape(ATTN_KV_HEADS, grp, BLOCK, 3 * BLOCK)
    key_pos = jnp.arange(nblk, dtype=jnp.int32)[:, None] * BLOCK - BLOCK + c_off[None, :]
    in_range = (key_pos >= 0) & (key_pos < s)
    mask = in_range[:, None, :] & (jnp.abs(rel) <= WINDOW)[None]

    scores = jnp.where(mask[None, :, None, None], scores + bias[None, None], NEG_INF)
    sink_b = sink.astype(jnp.float32).reshape(ATTN_KV_HEADS, grp)[None, None, :, :, None, None]
    m = jnp.maximum(jnp.max(scores, axis=-1, keepdims=True), sink_b)
    p = jnp.exp(scores - m)
    denom = jnp.sum(p, axis=-1, keepdims=True) + jnp.exp(sink_b - m)
    probs = (p / denom).astype(vb.dtype)
    out = jnp.einsum('bnkgqc,bnckd->bnqkgd', probs, vb)
    return out.reshape(b, s, ATTN_WIDTH)


def s5_scan(u, a_re, a_im, log_dt, b_re, b_im, c_re, c_im):
    dt = jnp.exp(log_dt)[:, None]
    mag = jnp.exp(a_re * dt)
    lb_re = mag * jnp.cos(a_im * dt)
    lb_im = mag * jnp.sin(a_im * dt)
    den = a_re * a_re + a_im * a_im
    nr = lb_re - 1.0
    coef_re = (nr * a_re + lb_im * a_im) / den
    coef_im = (lb_im * a_re - nr * a_im) / den
    bb_re = coef_re[..., None] * b_re - coef_im[..., None] * b_im
    bb_im = coef_re[..., None] * b_im + coef_im[..., None] * b_re
    x_re = jnp.einsum('bsgh,gph->bsgp', u, bb_re)
    x_im = jnp.einsum('bsgh,gph->bsgp', u, bb_im)
    s = u.shape[1]
    la_re = jnp.broadcast_to(lb_re[None, None], (1, s) + lb_re.shape)
    la_im = jnp.broadcast_to(lb_im[None, None], (1, s) + lb_im.shape)

    def combine(e1, e2):
        a1r, a1i, b1r, b1i = e1
        a2r, a2i, b2r, b2i = e2
        return (a2r * a1r - a2i * a1i,
                a2r * a1i + a2i * a1r,
                a2r * b1r - a2i * b1i + b2r,
                a2r * b1i + a2i * b1r + b2i)

    _, _, h_re, h_im = lax.associative_scan(combine, (la_re, la_im, x_re, x_im), axis=1)
    return jnp.einsum('bsgp,ghp->bsgh', h_re, c_re) - jnp.einsum('bsgp,ghp->bsgh', h_im, c_im)


def s5_mixer(u, a_re, a_im, log_dt, b_re, b_im, c_re, c_im, d_skip, glu_w, glu_b):
    b, s, _ = u.shape
    f = lambda t: t.astype(jnp.float32)
    uf = f(u).reshape(b, s, SSM_GROUPS, SSM_GROUP)
    y_fwd = s5_scan(uf, f(a_re[0]), f(a_im[0]), f(log_dt[0]), f(b_re[0]), f(b_im[0]), f(c_re[0]), f(c_im[0]))
    y_bwd = s5_scan(uf[:, ::-1], f(a_re[1]), f(a_im[1]), f(log_dt[1]), f(b_re[1]), f(b_im[1]),
                    f(c_re[1]), f(c_im[1]))[:, ::-1]
    y = (y_fwd + y_bwd).reshape(b, s, SSM_WIDTH) + f(d_skip) * f(u)
    g = jax.nn.gelu(y)
    out = g * jax.nn.sigmoid(g @ f(glu_w) + f(glu_b))
    return out.astype(u.dtype)


def gmlp_mixer(h, w_in, ln_g, ln_b, w_s, b_s):
    b, s, _ = h.shape
    z = jax.nn.gelu(h @ w_in)
    u, v = jnp.split(z, 2, axis=-1)
    vf = v.astype(jnp.float32)
    mu = jnp.mean(vf, axis=-1, keepdims=True)
    var = jnp.mean(jnp.square(vf - mu), axis=-1, keepdims=True)
    vn = ((vf - mu) * lax.rsqrt(var + EPS) * ln_g.astype(jnp.float32) + ln_b.astype(jnp.float32)).astype(v.dtype)
    vc = vn.reshape(b, s // GMLP_CHUNK, GMLP_CHUNK, GMLP_HEADS, GMLP_HEAD_DIM)
    mixed = jnp.einsum('hts,bnshd->bnthd', w_s, vc) + b_s.T[None, None, :, :, None]
    return u * mixed.reshape(b, s, GMLP_WIDTH)


def expert_choice_ffn(h, router, w1, w3, w2):
    b, s, _ = h.shape
    cap = CAPACITY_FACTOR * s // N_EXPERTS
    aff = jax.nn.softmax((h @ router).astype(jnp.float32), axis=-1)
    gate, idx = lax.top_k(jnp.swapaxes(aff, 1, 2), cap)
    bidx = jnp.arange(b)[:, None, None]
    xs = h[bidx, idx]
    hid = jax.nn.silu(jnp.einsum('becd,edf->becf', xs, w1)) * jnp.einsum('becd,edf->becf', xs, w3)
    ys = jnp.einsum('becf,efd->becd', hid, w2) * gate[..., None].astype(h.dtype)
    return jnp.zeros_like(h).at[bidx, idx].add(ys)


def setup_inputs(seed: int = 0) -> dict:
    key = jax.random.key(seed)
    ks = jax.random.split(key, 32)
    f32 = jnp.float32

    def nrm(k, shape, scale):
        return jax.random.normal(k, shape, f32) * scale

    dir_shape = (N_EVEN, 2, SSM_GROUPS)
    n_idx = jnp.arange(SSM_STATE, dtype=f32)
    return {
        'x': nrm(ks[0], (BATCH, SEQ, D_MODEL), 1.0),
        'rel_bias': nrm(ks[1], (REL_BUCKETS, ATTN_HEADS), 0.5),
        'mix_norm': 1.0 + nrm(ks[2], (DEPTH, D_MODEL), 0.02),
        'ffn_norm': 1.0 + nrm(ks[3], (DEPTH, D_MODEL), 0.02),
        'final_norm': 1.0 + nrm(ks[4], (D_MODEL,), 0.02),
        'even_w_in': nrm(ks[5], (N_EVEN, D_MODEL, EVEN_IN), D_MODEL ** -0.5),
        'attn_sink': nrm(ks[6], (N_EVEN, ATTN_HEADS), 1.0),
        'ssm_a_re': -0.5 + nrm(ks[7], dir_shape + (SSM_STATE,), 0.01),
        'ssm_a_im': math.pi * n_idx + nrm(ks[8], dir_shape + (SSM_STATE,), 0.01),
        'ssm_log_dt': jax.random.uniform(ks[9], dir_shape, f32, math.log(1e-3), math.log(1e-1)),
        'ssm_b_re': nrm(ks[10], dir_shape + (SSM_STATE, SSM_GROUP), (2 * SSM_GROUP) ** -0.5),
        'ssm_b_im': nrm(ks[11], dir_shape + (SSM_STATE, SSM_GROUP), (2 * SSM_GROUP) ** -0.5),
        'ssm_c_re': nrm(ks[12], dir_shape + (SSM_GROUP, SSM_STATE), (2 * SSM_STATE) ** -0.5),
        'ssm_c_im': nrm(ks[13], dir_shape + (SSM_GROUP, SSM_STATE), (2 * SSM_STATE) ** -0.5),
        'ssm_d': nrm(ks[14], (N_EVEN, SSM_WIDTH), 1.0),
        'glu_w': nrm(ks[15], (N_EVEN, SSM_WIDTH, SSM_WIDTH), SSM_WIDTH ** -0.5),
        'glu_b': nrm(ks[16], (N_EVEN, SSM_WIDTH), 0.02),
        'even_w_out': nrm(ks[17], (N_EVEN, ATTN_WIDTH + SSM_WIDTH, D_MODEL), (ATTN_WIDTH + SSM_WIDTH) ** -0.5),
        'odd_w_in': nrm(ks[18], (N_ODD, D_MODEL, 2 * GMLP_WIDTH), D_MODEL ** -0.5),
        'sgu_ln_g': 1.0 + nrm(ks[19], (N_ODD, GMLP_WIDTH), 0.02),
        'sgu_ln_b': nrm(ks[20], (N_ODD, GMLP_WIDTH), 0.02),
        'sgu_w': nrm(ks[21], (N_ODD, GMLP_HEADS, GMLP_CHUNK, GMLP_CHUNK), GMLP_CHUNK ** -0.5),
        'sgu_b': 1.0 + nrm(ks[22], (N_ODD, GMLP_HEADS, GMLP_CHUNK), 0.02),
        'odd_w_out': nrm(ks[23], (N_ODD, GMLP_WIDTH, D_MODEL), GMLP_WIDTH ** -0.5),
        'router': nrm(ks[24], (DEPTH, D_MODEL, N_EXPERTS), D_MODEL ** -0.5),
        'moe_w1': nrm(ks[25], (DEPTH, N_EXPERTS, D_MODEL, EXPERT_FF), D_MODEL ** -0.5),
        'moe_w3': nrm(ks[26], (DEPTH, N_EXPERTS, D_MODEL, EXPERT_FF), D_MODEL ** -0.5),
        'moe_w2': nrm(ks[27], (DEPTH, N_EXPERTS, EXPERT_FF, D_MODEL), EXPERT_FF ** -0.5),
    }


def reference(x, rel_bias, mix_norm, ffn_norm, final_norm, even_w_in, attn_sink,
              ssm_a_re, ssm_a_im, ssm_log_dt, ssm_b_re, ssm_b_im, ssm_c_re, ssm_c_im,
              ssm_d, glu_w, glu_b, even_w_out, odd_w_in, sgu_ln_g, sgu_ln_b, sgu_w, sgu_b,
              odd_w_out, router, moe_w1, moe_w3, moe_w2):
    b, s, _ = x.shape
    for layer in range(DEPTH):
        h = rms_norm(x, mix_norm[layer])
        i = layer // 2
        if layer % 2 == 0:
            proj = h @ even_w_in[i]
            q, k, v, u = jnp.split(proj, [ATTN_WIDTH, ATTN_WIDTH + KV_WIDTH, ATTN_WIDTH + 2 * KV_WIDTH], axis=-1)
            attn = windowed_attention(q.reshape(b, s, ATTN_HEADS, HEAD_DIM),
                                      k.reshape(b, s, ATTN_KV_HEADS, HEAD_DIM),
                                      v.reshape(b, s, ATTN_KV_HEADS, HEAD_DIM),
                                      attn_sink[i], rel_bias)
            ssm = s5_mixer(u, ssm_a_re[i], ssm_a_im[i], ssm_log_dt[i], ssm_b_re[i], ssm_b_im[i],
                           ssm_c_re[i], ssm_c_im[i], ssm_d[i], glu_w[i], glu_b[i])
            x = x + jnp.concatenate([attn, ssm], axis=-1) @ even_w_out[i]
        else:
            g = gmlp_mixer(h, odd_w_in[i], sgu_ln_g[i], sgu_ln_b[i], sgu_w[i], sgu_b[i])
            x = x + g @ odd_w_out[i]
        h = rms_norm(x, ffn_norm[layer])
        x = x + expert_choice_ffn(h, router[layer], moe_w1[layer], moe_w3[layer], moe_w2[layer])
    return rms_norm(x, final_norm)
```

```python
import numpy as np
import concourse.bass as bass
import concourse.mybir as mybir
from concourse.bass_utils import run_bass_kernel_spmd

F32 = mybir.dt.float32
BF16 = mybir.dt.bfloat16
U32 = mybir.dt.uint32
I32 = mybir.dt.int32
AF = mybir.ActivationFunctionType
ALU = mybir.AluOpType
AX = mybir.AxisListType

SAME_ENGINE_SYNC = True
NDMA = 6


class Buf:
    def __init__(self, t, name):
        self.t = t
        self.name = name
        self.w = None
        self.r = {}

    def __getitem__(self, k):
        return self.t[k]


class Eng:
    def __init__(self, K, name, h, dma=False):
        self.K = K
        self.name = name
        self.h = h
        self.sem = K.newsem(name)
        self.n = 0
        self.seen = {}
        self.dn = 0
        self.dsem = [K.newsem(f"{name}_d{i}") for i in range(NDMA)] if dma else []

    def wait(self, tok):
        if tok is None:
            return
        sem, val = tok
        if self.seen.get(id(sem), 0) >= val:
            return
        self.h.wait_ge(sem, val)
        self.seen[id(sem)] = val


class K:
    def __init__(self, nc, stack):
        self.nc = nc
        self.stack = stack
        self.pe = Eng(self, "pe", nc.tensor)
        self.act = Eng(self, "act", nc.scalar, dma=True)
        self.dve = Eng(self, "dve", nc.vector)
        self.pool = Eng(self, "pool", nc.gpsimd, dma=True)
        self.sp = Eng(self, "sp", nc.sync, dma=True)
        self.out_toks = []
        self.nb = 0

    def newsem(self, name):
        return self.stack.enter_context(self.nc.semaphore(name))

    def sb(self, shape, dt, name=None):
        self.nb += 1
        name = name or f"b{self.nb}"
        t = self.stack.enter_context(self.nc.sbuf_tensor(name, list(shape), dt))
        return Buf(t, name)

    def ps(self, shape, dt, name=None):
        self.nb += 1
        name = name or f"p{self.nb}"
        t = self.stack.enter_context(self.nc.psum_tensor(name, list(shape), dt))
        return Buf(t, name)

    def _deps(self, eng, R, W):
        for b in R:
            self._w1(eng, b.w)
        for b in W:
            self._w1(eng, b.w)
            for sem_id, (sem, val) in list(b.r.items()):
                self._w1(eng, (sem, val))

    def _w1(self, eng, tok):
        if tok is None:
            return
        if tok[0] is eng.sem:
            if eng is self.pe or not SAME_ENGINE_SYNC:
                return
        eng.wait(tok)

    def _mark(self, tok, R, W):
        for b in R:
            b.r[id(tok[0])] = tok
        for b in W:
            b.w = tok
            b.r = {}

    def op(self, eng, fn, R=(), W=()):
        self._deps(eng, R, W)
        inst = fn(eng.h)
        eng.n += 1
        inst.then_inc(eng.sem, 1)
        tok = (eng.sem, eng.n)
        self._mark(tok, R, W)
        return tok

    def dma(self, q, out, in_, R=(), W=(), is_output=False, indirect=None, **kw):
        self._deps(q, R, W)
        slot = q.dn % NDMA
        rnd = q.dn // NDMA
        if rnd > 0:
            q.wait((q.dsem[slot], 16 * rnd))
        if indirect is None:
            inst = q.h.dma_start(out=out, in_=in_, **kw)
        else:
            inst = q.h.indirect_dma_start(out=out, in_=in_, **indirect, **kw)
        inst.then_inc(q.dsem[slot], 16)
        tok = (q.dsem[slot], 16 * (rnd + 1))
        q.dn += 1
        self._mark(tok, R, W)
        if is_output:
            self.out_toks.append(tok)
        return tok

    def finish(self):
        for q in (self.sp, self.act, self.pool):
            for slot in range(NDMA):
                cnt = (q.dn - slot + NDMA - 1) // NDMA if q.dn > slot else 0
                if cnt > 0:
                    self.sp.wait((q.dsem[slot], 16 * cnt))


def make_identity(k, n=128, dt=F32):
    ones = k.sb([n, n], F32, "id_ones")
    ident = k.sb([n, n], F32, "ident_f")
    k.op(k.pool, lambda g: g.memset(ones[:], 1.0), W=[ones])
    k.op(k.pool, lambda g: g.affine_select(out=ident[:], in_=ones[:], pattern=[[1, n]], compare_op=ALU.is_equal,
                                           fill=0.0, base=0, channel_multiplier=-1), R=[ones], W=[ident])
    if dt == F32:
        return ident
    idb = k.sb([n, n], dt, "ident_b")
    k.op(k.dve, lambda v: v.tensor_copy(out=idb[:], in_=ident[:]), R=[ident], W=[idb])
    return idb


def build_moe(S=4096, D=2048, F=2048, EL=8, C=512, debug=False):
    from contextlib import ExitStack
    nc = bass.Bass("TRN2", target_bir_lowering=False)
    aff = nc.dram_tensor("aff", [EL, S], F32, kind="ExternalInput").ap()
    h = nc.dram_tensor("h", [S, D], F32, kind="ExternalInput").ap()
    w1 = nc.dram_tensor("w1", [EL, D, F], F32, kind="ExternalInput").ap()
    w3 = nc.dram_tensor("w3", [EL, D, F], F32, kind="ExternalInput").ap()
    w2 = nc.dram_tensor("w2", [EL, F, D], F32, kind="ExternalInput").ap()
    y = nc.dram_tensor("y", [S, D], F32, kind="ExternalOutput").ap()
    NB = D // 512
    y4 = y.rearrange("s (b c) -> (s b) c", c=512)
    J = C // 128
    KT = D // 128
    FT = F // 128
    assert D == F
    with ExitStack() as st:
        k = K(nc, st)
        pe, act, dve, pool, sp = k.pe, k.act, k.dve, k.pool, k.sp
        identf = make_identity(k, 128, F32)
        identb = k.sb([128, 128], BF16, "identb")
        k.op(dve, lambda v: v.tensor_copy(out=identb[:], in_=identf[:]), R=[identf], W=[identb])

        zt = k.sb([128, D], F32, "zt")
        k.op(pool, lambda g: g.memset(zt[:], 0.0), W=[zt])
        ztoks = []
        for i in range(S // 128):
            ztoks.append(k.dma(sp, y[i * 128:(i + 1) * 128, :], zt[:], R=[zt]))

        affw = k.sb([EL, S], F32, "affw")
        vals = k.sb([EL, C], F32, "vals")
        idxu = k.sb([EL, C], U32, "idxu")
        idxf = k.sb([EL, C], F32, "idxf")
        k.dma(act, affw[:], aff[:, :], W=[affw])
        for r in range(C // 8):
            sl = slice(r * 8, r * 8 + 8)
            k.op(dve, lambda v: v.max(out=vals[:, sl], in_=affw[:]), R=[affw], W=[vals])
            k.op(dve, lambda v: v.max_index(out=idxu[:, sl], in_max=vals[:, sl], in_values=affw[:]), R=[affw, vals], W=[idxu])
            k.op(dve, lambda v: v.match_replace(out=affw[:], in_to_replace=vals[:, sl], in_values=affw[:], imm_value=-1.0),
                 R=[vals], W=[affw])
        k.op(dve, lambda v: v.tensor_copy(out=idxf[:], in_=idxu[:]), R=[idxu], W=[idxf])
        ptp = k.ps([128, 2 * J * EL], F32, "ptp")
        idxTf = k.sb([128, J * EL], F32, "idxTf")
        gT = k.sb([128, J * EL], F32, "gT")
        for j in range(J):
            k.op(pe, lambda t: t.transpose(out=ptp[:, j * EL:(j + 1) * EL], in_=idxf[:, j * 128:(j + 1) * 128], identity=identf[0:EL, 0:EL]),
                 R=[idxf, identf], W=[ptp])
            k.op(pe, lambda t: t.transpose(out=ptp[:, (J + j) * EL:(J + j + 1) * EL], in_=vals[:, j * 128:(j + 1) * 128], identity=identf[0:EL, 0:EL]),
                 R=[vals, identf], W=[ptp])
        k.op(dve, lambda v: v.tensor_copy(out=idxTf[:], in_=ptp[:, 0:J * EL]), R=[ptp], W=[idxTf])
        k.op(dve, lambda v: v.tensor_copy(out=gT[:], in_=ptp[:, J * EL:2 * J * EL]), R=[ptp], W=[gT])
        idxT = k.sb([128, J * EL], U32, "idxT")
        k.op(dve, lambda v: v.tensor_copy(out=idxT[:], in_=idxTf[:]), R=[idxTf], W=[idxT])
        idx4 = []
        for b in range(NB):
            t = k.sb([128, J * EL], U32, f"idx4_{b}")
            k.op(dve, lambda v: v.tensor_scalar(out=t[:], in0=idxTf[:], scalar1=float(NB), scalar2=float(b), op0=ALU.mult, op1=ALU.add),
                 R=[idxTf], W=[t])
            idx4.append(t)

        xs = [k.sb([128, D], F32, f"xs{i}") for i in range(2)]
        xsb = [k.sb([128, D], BF16, f"xsb{i}") for i in range(2)]
        xsT = k.sb([128, KT, C], BF16, "xsT")
        hidT = k.sb([128, FT, C], BF16, "hidT")
        sbuf_s = [k.sb([128, C], F32, f"silu{i}") for i in range(2)]
        stage = [k.sb([128, KT // 2, 512], F32, f"stage{i}") for i in range(3)]
        wblk = [k.sb([128, KT, 512], BF16, f"wblk{i}") for i in range(2)]
        ysb = [k.sb([128, 512], F32, f"ys{i}") for i in range(3)]
        ptr = [k.ps([128, 1024], BF16, f"ptr{i}") for i in range(2)]
        pacc = [k.ps([128, 512], F32, f"pacc{i}") for i in range(4)]
        ybuf = [Buf(None, f"ybuf{b}") for b in range(NB)]
        cnt = {"stage": 0, "wblk": 0, "pacc": 0, "ys": 0, "silu": 0, "xs": 0, "ptr": 0, "cast": 0}

        def nxt(name, lst):
            i = cnt[name] % len(lst)
            cnt[name] += 1
            return lst[i]

        def load_block(src):
            wb = nxt("wblk", wblk)
            srcv = src.rearrange("(kt p) f -> p kt f", p=128)
            for hh in range(2):
                sg = nxt("stage", stage)
                k.dma(sp, sg[:], srcv[:, hh * (KT // 2):(hh + 1) * (KT // 2), :], W=[sg])
                ce = act if cnt["cast"] % 2 == 0 else pool
                cnt["cast"] += 1
                if ce is act:
                    k.op(act, lambda a: a.copy(out=wb[:, hh * (KT // 2):(hh + 1) * (KT // 2), :], in_=sg[:]), R=[sg], W=[wb])
                else:
                    k.op(pool, lambda g: g.tensor_copy(out=wb[:, hh * (KT // 2):(hh + 1) * (KT // 2), :], in_=sg[:]), R=[sg], W=[wb])
            return wb

        first_scatter = True
        for e in range(EL):
            for j in range(J):
                col = j * EL + e
                x_ = nxt("xs", xs)
                xb_ = xsb[(cnt["xs"] - 1) % 2]
                k.dma(pool, x_[:], h[:, :], R=[idxT], W=[x_],
                      indirect=dict(out_offset=None, in_offset=bass.IndirectOffsetOnAxis(ap=idxT[:, col:col + 1], axis=0)))
                k.op(act, lambda a: a.copy(out=xb_[:], in_=x_[:]), R=[x_], W=[xb_])
                G = min(8, KT)
                for half in range(KT // G):
                    p_ = nxt("ptr", ptr)
                    for i in range(G):
                        dt_ = half * G + i
                        k.op(pe, lambda t: t.transpose(out=p_[:, i * 128:(i + 1) * 128], in_=xb_[:, dt_ * 128:(dt_ + 1) * 128], identity=identb[:]),
                             R=[xb_, identb], W=[p_])
                    k.op(dve, lambda v: v.tensor_copy(out=xsT[:, half * G:(half + 1) * G, j * 128:(j + 1) * 128],
                                                      in_=p_[:, 0:G * 128].rearrange("p (a b) -> p a b", b=128)), R=[p_], W=[xsT])
            for fb in range(F // 512):
                wa = load_block(w1[e, :, fb * 512:(fb + 1) * 512])
                wc = load_block(w3[e, :, fb * 512:(fb + 1) * 512])
                for ft in range(4):
                    pa = nxt("pacc", pacc)
                    pb = nxt("pacc", pacc)
                    for kt in range(KT):
                        k.op(pe, lambda t: t.matmul(pa[:, 0:C], lhsT=wa[:, kt, ft * 128:(ft + 1) * 128], rhs=xsT[:, kt, :], start=(kt == 0), stop=(kt == KT - 1)),
                             R=[wa, xsT], W=[pa])
                    for kt in range(KT):
                        k.op(pe, lambda t: t.matmul(pb[:, 0:C], lhsT=wc[:, kt, ft * 128:(ft + 1) * 128], rhs=xsT[:, kt, :], start=(kt == 0), stop=(kt == KT - 1)),
                             R=[wc, xsT], W=[pb])
                    s_ = nxt("silu", sbuf_s)
                    k.op(act, lambda a: a.activation(out=s_[:], in_=pa[:, 0:C], func=AF.Silu), R=[pa], W=[s_])
                    k.op(dve, lambda v: v.tensor_tensor(out=hidT[:, fb * 4 + ft, :], in0=s_[:], in1=pb[:, 0:C], op=ALU.mult), R=[s_, pb], W=[hidT])
            for db in range(NB):
                wd = load_block(w2[e, :, db * 512:(db + 1) * 512])
                for j in range(J):
                    col = j * EL + e
                    pa = nxt("pacc", pacc)
                    for ft in range(FT):
                        k.op(pe, lambda t: t.matmul(pa[:], lhsT=hidT[:, ft, j * 128:(j + 1) * 128], rhs=wd[:, ft, :], start=(ft == 0), stop=(ft == FT - 1)),
                             R=[hidT, wd], W=[pa])
                    y_ = nxt("ys", ysb)
                    k.op(dve, lambda v: v.tensor_scalar(out=y_[:], in0=pa[:], scalar1=gT[:, col:col + 1], scalar2=None, op0=ALU.mult), R=[pa, gT], W=[y_])
                    if first_scatter:
                        for tk in ztoks:
                            pool.wait(tk)
                        first_scatter = False
                    k.dma(pool, y4[:, :], y_[:], R=[y_, idx4[db]], W=[ybuf[db]],
                          indirect=dict(out_offset=bass.IndirectOffsetOnAxis(ap=idx4[db][:, col:col + 1], axis=0), in_offset=None,
                                        compute_op=ALU.add))
        if debug:
            def dump(name, buf, shape, dt=F32):
                d = nc.dram_tensor(name, list(shape), dt, kind="ExternalOutput").ap()
                k.dma(sp, d, buf[:], R=[buf])
            dump("d_idxTf", idxTf, [128, J * EL])
            dump("d_gT", gT, [128, J * EL])
            dump("d_idx4", idx4[0], [128, J * EL], U32)
            dump("d_xs", xs[0], [128, D])
            dump("d_xsT", xsT, [128, KT, C], BF16)
            dump("d_hidT", hidT, [128, FT, C], BF16)
            dump("d_ys", ysb[0], [128, 512])
        k.finish()
    return nc


class Common:
    def __init__(self, k, N, KT=16):
        self.k = k
        self.N = N
        self.KT = KT
        self.ones = k.sb([128, 128], BF16, "ones_bf")
        k.op(k.pool, lambda g: g.memset(self.ones[:], 1.0), W=[self.ones])
        self.identf = make_identity(k, 128, F32)
        self.stage = [k.sb([128, 4, 512], F32, f"cstage{i}") for i in range(2)]
        self.nstage = 0
        self.ncast = 0
        self.sq = k.sb([128, KT, N], BF16, "sq")
        self.rstd = k.sb([128, N], F32, "rstd")
        self.rtmp = k.sb([128, N], F32, "rtmp")

    def load_w(self, dst, src, ncols, kt_total, col0=0):
        k = self.k
        srcv = src.rearrange("(kt p) f -> p kt f", p=128)
        for c0 in range(0, ncols, 512):
            cw = min(512, ncols - c0)
            for k0 in range(0, kt_total, 4):
                kw_ = min(4, kt_total - k0)
                sg = self.stage[self.nstage % 2]
                self.nstage += 1
                k.dma(k.sp, sg[:, 0:kw_, 0:cw], srcv[:, k0:k0 + kw_, col0 + c0:col0 + c0 + cw], W=[sg])
                if self.ncast % 2 == 0:
                    k.op(k.act, lambda a: a.copy(out=dst[:, k0:k0 + kw_, c0:c0 + cw], in_=sg[:, 0:kw_, 0:cw]), R=[sg], W=[dst])
                else:
                    k.op(k.pool, lambda g: g.tensor_copy(out=dst[:, k0:k0 + kw_, c0:c0 + cw], in_=sg[:, 0:kw_, 0:cw]), R=[sg], W=[dst])
                self.ncast += 1

    def rmsnorm(self, xt, gl, ps, out, n=None, D=2048, eps=1e-6):
        k = self.k
        n = n or self.N
        KT = self.KT
        k.op(k.act, lambda a: a.activation(out=self.sq[:, :, 0:n], in_=xt[:, :, 0:n], func=AF.Square), R=[xt], W=[self.sq])
        for kt in range(KT):
            k.op(k.pe, lambda t: t.matmul(ps[:, 0:n], lhsT=self.ones[:], rhs=self.sq[:, kt, 0:n], start=(kt == 0), stop=(kt == KT - 1)),
                 R=[self.ones, self.sq], W=[ps])
        k.op(k.act, lambda a: a.activation(out=self.rtmp[:, 0:n], in_=ps[:, 0:n], func=AF.Sqrt, scale=1.0 / D, bias=self.epsb[:]),
             R=[ps, self.epsb], W=[self.rtmp])
        k.op(k.dve, lambda v: v.reciprocal(out=self.rstd[:, 0:n], in_=self.rtmp[:, 0:n]), R=[self.rtmp], W=[self.rstd])
        for kt in range(KT):
            k.op(k.dve, lambda v: v.scalar_tensor_tensor(out=out[:, kt, 0:n], in0=xt[:, kt, 0:n], scalar=gl[:, kt:kt + 1], in1=self.rstd[:, 0:n],
                                                         op0=ALU.mult, op1=ALU.mult), R=[xt, gl, self.rstd], W=[out])

    def setup_eps(self, eps=1e-6):
        k = self.k
        self.epsb = k.sb([128, 1], F32, "epsb")
        k.op(k.pool, lambda g: g.memset(self.epsb[:], eps), W=[self.epsb])


def tail_norm_router(k, cm, x1, fng, rw, ps_a, ps_b, h2, lg, affsb, x1T, h2T, affT, t0, N):
    KT = 16
    k.dma(k.sp, x1T.rearrange("(kt p) t -> p kt t", p=128)[:, :, t0:t0 + N], x1[:], R=[x1])
    cm.rmsnorm(x1, fng, ps_a, h2)
    k.dma(k.sp, h2T.rearrange("(kt p) t -> p kt t", p=128)[:, :, t0:t0 + N], h2[:], R=[h2])
    E = 16
    for kt in range(KT):
        k.op(k.pe, lambda t: t.matmul(ps_b[0:E, 0:N], lhsT=rw[:, kt, :], rhs=h2[:, kt, :], start=(kt == 0), stop=(kt == KT - 1)),
             R=[rw, h2], W=[ps_b])
    k.op(k.dve, lambda v: v.tensor_copy(out=lg[:], in_=ps_b[0:E, 0:N]), R=[ps_b], W=[lg])
    nsub = N // 128
    for c in range(nsub):
        k.op(k.pe, lambda t: t.transpose(out=ps_a[:, c * E:(c + 1) * E], in_=lg[:, c * 128:(c + 1) * 128], identity=cm.identf[0:E, 0:E]),
             R=[lg, cm.identf], W=[ps_a])
    mx = cm.mx
    k.op(k.dve, lambda v: v.tensor_reduce(out=mx[:, 0:nsub], in_=ps_a[:, 0:nsub * E].rearrange("p (c e) -> p c e", e=E), axis=AX.X, op=ALU.max),
         R=[ps_a], W=[mx])
    k.op(k.dve, lambda v: v.tensor_scalar(out=mx[:, 0:nsub], in0=mx[:, 0:nsub], scalar1=-1.0, scalar2=None, op0=ALU.mult), R=[mx], W=[mx])
    for c in range(nsub):
        k.op(k.act, lambda a: a.activation(out=affsb[:, c, :], in_=ps_a[:, c * E:(c + 1) * E], func=AF.Exp, bias=mx[:, c:c + 1],
                                           accum_out=cm.sm[:, c:c + 1]), R=[ps_a, mx], W=[affsb, cm.sm])
    k.op(k.dve, lambda v: v.reciprocal(out=cm.sm[:, 0:nsub], in_=cm.sm[:, 0:nsub]), R=[cm.sm], W=[cm.sm])
    for c in range(nsub):
        k.op(k.dve, lambda v: v.tensor_scalar(out=affsb[:, c, :], in0=affsb[:, c, :], scalar1=cm.sm[:, c:c + 1], scalar2=None, op0=ALU.mult),
             R=[affsb, cm.sm], W=[affsb])
    k.dma(k.sp, affT[t0:t0 + N, :].rearrange("(c p) e -> p c e", p=128), affsb[:, 0:nsub, :], R=[affsb])


def build_c(T=2048, N=256):
    from contextlib import ExitStack
    nc = bass.Bass("TRN2", target_bir_lowering=False)
    D = 2048
    xT = nc.dram_tensor("xT", [D, T], F32, kind="ExternalInput").ap()
    attnT = nc.dram_tensor("attnT", [1024, T], BF16, kind="ExternalInput").ap()
    gsT = nc.dram_tensor("gsT", [1024, T], F32, kind="ExternalInput").ap()
    glu_w = nc.dram_tensor("glu_w", [1024, 1024], F32, kind="ExternalInput").ap()
    glu_bl = nc.dram_tensor("glu_bl", [128, 8], F32, kind="ExternalInput").ap()
    w_out = nc.dram_tensor("w_out", [D, D], F32, kind="ExternalInput").ap()
    fngl = nc.dram_tensor("fngl", [128, 16], F32, kind="ExternalInput").ap()
    router = nc.dram_tensor("router", [D, 16], F32, kind="ExternalInput").ap()
    x1T = nc.dram_tensor("x1T", [D, T], F32, kind="ExternalOutput").ap()
    h2T = nc.dram_tensor("h2T", [D, T], F32, kind="ExternalOutput").ap()
    affT = nc.dram_tensor("affT", [T, 16], F32, kind="ExternalOutput").ap()
    with ExitStack() as st:
        k = K(nc, st)
        pe, act, dve, pool, sp = k.pe, k.act, k.dve, k.pool, k.sp
        cm = Common(k, N)
        cm.setup_eps()
        cm.mx = k.sb([128, 8], F32, "mx")
        cm.sm = k.sb([128, 8], F32, "sm")
        gw = k.sb([128, 8, 1024], BF16, "gw")
        wo = k.sb([128, 16, D], BF16, "wo")
        gb = k.sb([128, 8], F32, "gb")
        fng = k.sb([128, 16], F32, "fng")
        rw = k.sb([128, 16, 16], F32, "rw")
        k.dma(act, gb[:], glu_bl[:, :], W=[gb])
        k.dma(act, fng[:], fngl[:, :], W=[fng])
        k.dma(act, rw[:], router.rearrange("(kt p) e -> p kt e", p=128), W=[rw])
        cm.load_w(gw, glu_w, 1024, 8)
        cm.load_w(wo, w_out, D, 16)
        xt = [k.sb([128, 16, N], F32, f"xt{i}") for i in range(2)]
        gs = k.sb([128, 8, N], F32, "gs")
        gsb = k.sb([128, 8, N], BF16, "gsb")
        cat = k.sb([128, 16, N], BF16, "cat")
        sig = [k.sb([128, N], F32, f"sig{i}") for i in range(2)]
        h2 = k.sb([128, 16, N], F32, "h2")
        lg = k.sb([16, N], F32, "lg")
        affsb = k.sb([128, 4, 16], F32, "affsb")
        pss = [k.ps([128, 512], F32, f"ps{i}") for i in range(6)]
        npz = [0]

        def nps():
            npz[0] += 1
            return pss[npz[0] % 4]
        xv = xT.rearrange("(kt p) t -> p kt t", p=128)
        av = attnT.rearrange("(kt p) t -> p kt t", p=128)
        gv = gsT.rearrange("(kt p) t -> p kt t", p=128)
        for ti in range(T // N):
            t0 = ti * N
            x_ = xt[ti % 2]
            k.dma(sp, x_[:], xv[:, :, t0:t0 + N], W=[x_])
            k.dma(sp, gs[:], gv[:, :, t0:t0 + N], W=[gs])
            k.dma(act, cat[:, 0:8, :], av[:, :, t0:t0 + N], W=[cat])
            k.op(act, lambda a: a.copy(out=gsb[:], in_=gs[:]), R=[gs], W=[gsb])
            for m in range(8):
                p_ = nps()
                for kt in range(8):
                    k.op(pe, lambda t: t.matmul(p_[:, 0:N], lhsT=gw[:, kt, m * 128:(m + 1) * 128], rhs=gsb[:, kt, :], start=(kt == 0), stop=(kt == 7)),
                         R=[gw, gsb], W=[p_])
                s_ = sig[m % 2]
                k.op(act, lambda a: a.activation(out=s_[:], in_=p_[:, 0:N], func=AF.Sigmoid, bias=gb[:, m:m + 1]), R=[p_, gb], W=[s_])
                k.op(dve, lambda v: v.tensor_tensor(out=cat[:, 8 + m, :], in0=gs[:, m, :], in1=s_[:], op=ALU.mult), R=[gs, s_], W=[cat])
            for m in range(16):
                p_ = nps()
                for kt in range(16):
                    k.op(pe, lambda t: t.matmul(p_[:, 0:N], lhsT=wo[:, kt, m * 128:(m + 1) * 128], rhs=cat[:, kt, :], start=(kt == 0), stop=(kt == 15)),
                         R=[wo, cat], W=[p_])
                k.op(dve, lambda v: v.tensor_tensor(out=x_[:, m, :], in0=x_[:, m, :], in1=p_[:, 0:N], op=ALU.add), R=[p_], W=[x_])
            tail_norm_router(k, cm, x_, fng, rw, pss[4], pss[5], h2, lg, affsb, x1T, h2T, affT, t0, N)
        k.finish()
    return nc


class BlockLoader:
    def __init__(self, k, KT=16, nblk=2, nstage=2):
        self.k = k
        self.KT = KT
        self.stage = [k.sb([128, 4, 512], F32, f"bstage{i}") for i in range(nstage)]
        self.blk = [k.sb([128, KT, 512], BF16, f"bblk{i}") for i in range(nblk)]
        self.ns = 0
        self.nb = 0
        self.nc_ = 0

    def load(self, src, kt_total=None):
        k = self.k
        kt_total = kt_total or self.KT
        wb = self.blk[self.nb % len(self.blk)]
        self.nb += 1
        srcv = src.rearrange("(kt p) f -> p kt f", p=128)
        for k0 in range(0, kt_total, 4):
            sg = self.stage[self.ns % len(self.stage)]
            self.ns += 1
            k.dma(k.sp, sg[:], srcv[:, k0:k0 + 4, :], W=[sg])
            if self.nc_ % 2 == 0:
                k.op(k.act, lambda a: a.copy(out=wb[:, k0:k0 + 4, :], in_=sg[:]), R=[sg], W=[wb])
            else:
                k.op(k.pool, lambda g: g.tensor_copy(out=wb[:, k0:k0 + 4, :], in_=sg[:]), R=[sg], W=[wb])
            self.nc_ += 1
        return wb


def build_e(T=2048, N=256):
    from contextlib import ExitStack
    nc = bass.Bass("TRN2", target_bir_lowering=False)
    D = 2048
    x1T = nc.dram_tensor("x1T", [D, T], F32, kind="ExternalInput").ap()
    paT = nc.dram_tensor("paT", [D, T], F32, kind="ExternalInput").ap()
    pbT = nc.dram_tensor("pbT", [D, T], F32, kind="ExternalInput").ap()
    mngl = nc.dram_tensor("mngl", [128, 16], F32, kind="ExternalInput").ap()
    w_in = nc.dram_tensor("w_in", [D, 2 * D], F32, kind="ExternalInput").ap()
    lng_bc = nc.dram_tensor("lng_bc", [128, D], F32, kind="ExternalInput").ap()
    lnb_bc = nc.dram_tensor("lnb_bc", [128, D], F32, kind="ExternalInput").ap()
    sgu_wT = nc.dram_tensor("sgu_wT", [128, 16, 128], F32, kind="ExternalInput").ap()
    sgu_b_bc = nc.dram_tensor("sgu_b_bc", [128, 16, 128], F32, kind="ExternalInput").ap()
    w_out = nc.dram_tensor("w_out", [D, D], F32, kind="ExternalInput").ap()
    fngl = nc.dram_tensor("fngl", [128, 16], F32, kind="ExternalInput").ap()
    router = nc.dram_tensor("router", [D, 16], F32, kind="ExternalInput").ap()
    x3T = nc.dram_tensor("x1T_out", [D, T], F32, kind="ExternalOutput").ap()
    h3T = nc.dram_tensor("h2T", [D, T], F32, kind="ExternalOutput").ap()
    affT = nc.dram_tensor("affT", [T, 16], F32, kind="ExternalOutput").ap()
    NCH = N // 128
    with ExitStack() as st:
        k = K(nc, st)
        pe, act, dve, pool, sp = k.pe, k.act, k.dve, k.pool, k.sp
        cm = Common(k, N)
        cm.setup_eps()
        cm.mx = k.sb([128, 8], F32, "mx")
        cm.sm = k.sb([128, 8], F32, "sm")
        bl = BlockLoader(k)
        mng = k.sb([128, 16], F32, "mng")
        fng = k.sb([128, 16], F32, "fng")
        rw = k.sb([128, 16, 16], F32, "rw")
        lng = k.sb([128, D], F32, "lng")
        lnb = k.sb([128, D], F32, "lnb")
        wsf = k.sb([128, 16, 128], F32, "wsf")
        wsT = k.sb([128, 16, 128], BF16, "wsT")
        sbb = k.sb([128, 16, 128], F32, "sbb")
        k.dma(act, mng[:], mngl[:, :], W=[mng])
        k.dma(act, fng[:], fngl[:, :], W=[fng])
        k.dma(act, rw[:], router.rearrange("(kt p) e -> p kt e", p=128), W=[rw])
        k.dma(act, lng[:], lng_bc[:, :], W=[lng])
        k.dma(act, lnb[:], lnb_bc[:, :], W=[lnb])
        k.dma(act, wsf[:], sgu_wT[:, :, :], W=[wsf])
        k.dma(act, sbb[:], sgu_b_bc[:, :, :], W=[sbb])
        k.op(dve, lambda v: v.tensor_copy(out=wsT[:], in_=wsf[:]), R=[wsf], W=[wsT])
        xa = k.sb([128, 16, N], F32, "xa")
        xb = k.sb([128, 16, N], F32, "xb")
        hT = k.sb([128, 16, N], BF16, "hT")
        uT = k.sb([128, 16, N], F32, "uT")
        vg = [k.sb([128, D], F32, f"vg{i}") for i in range(NCH)]
        vn = [k.sb([128, D], BF16, f"vn{i}") for i in range(NCH)]
        gated = k.sb([128, 16, N], BF16, "gated")
        tmp = [k.sb([128, N], F32, f"tmp{i}") for i in range(2)]
        st1 = k.sb([128, 16], F32, "st1")
        st2 = k.sb([128, 8], F32, "st2")
        junk = k.sb([128, D], BF16, "junk")
        lg = k.sb([16, N], F32, "lg")
        affsb = k.sb([128, 4, 16], F32, "affsb")
        pss = [k.ps([128, 512], F32, f"ps{i}") for i in range(6)]
        npz = [0]

        def nps():
            npz[0] += 1
            return pss[npz[0] % 4]
        v1 = x1T.rearrange("(kt p) t -> p kt t", p=128)
        va = paT.rearrange("(kt p) t -> p kt t", p=128)
        vb = pbT.rearrange("(kt p) t -> p kt t", p=128)
        for ti in range(T // N):
            t0 = ti * N
            k.dma(sp, xa[:], v1[:, :, t0:t0 + N], W=[xa])
            k.dma(sp, xb[:], va[:, :, t0:t0 + N], W=[xb])
            k.op(dve, lambda v: v.tensor_tensor(out=xa[:], in0=xa[:], in1=xb[:], op=ALU.add), R=[xb], W=[xa])
            k.dma(sp, xb[:], vb[:, :, t0:t0 + N], W=[xb])
            k.op(dve, lambda v: v.tensor_tensor(out=xa[:], in0=xa[:], in1=xb[:], op=ALU.add), R=[xb], W=[xa])
            cm.rmsnorm(xa, mng, pss[4], hT)
            for cb in range(4):
                wb = bl.load(w_in[:, cb * 512:(cb + 1) * 512])
                for mm in range(4):
                    m = cb * 4 + mm
                    p_ = nps()
                    for kt in range(16):
                        k.op(pe, lambda t: t.matmul(p_[:, 0:N], lhsT=wb[:, kt, mm * 128:(mm + 1) * 128], rhs=hT[:, kt, :], start=(kt == 0), stop=(kt == 15)),
                             R=[wb, hT], W=[p_])
                    k.op(act, lambda a: a.activation(out=uT[:, m, :], in_=p_[:, 0:N], func=AF.Gelu_apprx_tanh), R=[p_], W=[uT])
            for cb in range(4):
                wb = bl.load(w_in[:, D + cb * 512:D + (cb + 1) * 512])
                for c in range(NCH):
                    p_ = nps()
                    for kt in range(16):
                        k.op(pe, lambda t: t.matmul(p_[:], lhsT=hT[:, kt, c * 128:(c + 1) * 128], rhs=wb[:, kt, :], start=(kt == 0), stop=(kt == 15)),
                             R=[wb, hT], W=[p_])
                    k.op(act, lambda a: a.activation(out=vg[c][:, cb * 512:(cb + 1) * 512], in_=p_[:], func=AF.Gelu_apprx_tanh,
                                                     accum_out=st1[:, c * 4 + cb:c * 4 + cb + 1]), R=[p_], W=[vg[c], st1])
            for c in range(NCH):
                k.op(act, lambda a: a.activation(out=junk[:], in_=vg[c][:], func=AF.Square, accum_out=st2[:, 0:1]), R=[vg[c]], W=[junk, st2])
                k.op(dve, lambda v: v.tensor_reduce(out=st2[:, 1:2], in_=st1[:, c * 4:c * 4 + 4], axis=AX.X, op=ALU.add), R=[st1], W=[st2])
                k.op(dve, lambda v: v.tensor_scalar(out=st2[:, 2:3], in0=st2[:, 1:2], scalar1=1.0 / D, scalar2=None, op0=ALU.mult), R=[st2], W=[st2])
                k.op(dve, lambda v: v.tensor_tensor(out=st2[:, 3:4], in0=st2[:, 2:3], in1=st2[:, 2:3], op=ALU.mult), R=[st2], W=[st2])
                k.op(dve, lambda v: v.scalar_tensor_tensor(out=st2[:, 4:5], in0=st2[:, 0:1], scalar=1.0 / D, in1=st2[:, 3:4], op0=ALU.mult, op1=ALU.subtract),
                     R=[st2], W=[st2])
                k.op(act, lambda a: a.activation(out=st2[:, 5:6], in_=st2[:, 4:5], func=AF.Sqrt, bias=cm.epsb[:]), R=[st2, cm.epsb], W=[st2])
                k.op(dve, lambda v: v.reciprocal(out=st2[:, 6:7], in_=st2[:, 5:6]), R=[st2], W=[st2])
                k.op(dve, lambda v: v.tensor_scalar(out=vg[c][:], in0=vg[c][:], scalar1=st2[:, 2:3], scalar2=st2[:, 6:7], op0=ALU.subtract, op1=ALU.mult),
                     R=[st2], W=[vg[c]])
                k.op(dve, lambda v: v.tensor_tensor(out=vg[c][:], in0=vg[c][:], in1=lng[:], op=ALU.mult), R=[lng], W=[vg[c]])
                k.op(dve, lambda v: v.tensor_tensor(out=vn[c][:], in0=vg[c][:], in1=lnb[:], op=ALU.add), R=[lnb, vg[c]], W=[vn[c]])
            for hd in range(16):
                p_ = nps()
                for c in range(NCH):
                    k.op(pe, lambda t: t.matmul(p_[:, c * 128:(c + 1) * 128], lhsT=vn[c][:, hd * 128:(hd + 1) * 128], rhs=wsT[:, hd, :], start=True, stop=True),
                         R=[vn[c], wsT], W=[p_])
                t_ = tmp[hd % 2]
                k.op(dve, lambda v: v.tensor_tensor(out=t_[:].rearrange("p (c t) -> p c t", t=128), in0=p_[:, 0:N].rearrange("p (c t) -> p c t", t=128),
                                                    in1=sbb[:, hd, :].unsqueeze(1).to_broadcast([128, NCH, 128]), op=ALU.add), R=[p_, sbb], W=[t_])
                k.op(dve, lambda v: v.tensor_tensor(out=gated[:, hd, :], in0=t_[:], in1=uT[:, hd, :], op=ALU.mult), R=[t_, uT], W=[gated])
            for cb in range(4):
                wb = bl.load(w_out[:, cb * 512:(cb + 1) * 512])
                for mm in range(4):
                    m = cb * 4 + mm
                    p_ = nps()
                    for kt in range(16):
                        k.op(pe, lambda t: t.matmul(p_[:, 0:N], lhsT=wb[:, kt, mm * 128:(mm + 1) * 128], rhs=gated[:, kt, :], start=(kt == 0), stop=(kt == 15)),
                             R=[wb, gated], W=[p_])
                    k.op(dve, lambda v: v.tensor_tensor(out=xa[:, m, :], in0=xa[:, m, :], in1=p_[:, 0:N], op=ALU.add), R=[p_], W=[xa])
            tail_norm_router(k, cm, xa, fng, rw, pss[4], pss[5], xb, lg, affsb, x3T, h3T, affT, t0, N)
        k.finish()
    return nc


def build_g(T=2048, N=512):
    from contextlib import ExitStack
    nc = bass.Bass("TRN2", target_bir_lowering=False)
    D = 2048
    x1T = nc.dram_tensor("x1T", [D, T], F32, kind="ExternalInput").ap()
    paT = nc.dram_tensor("paT", [D, T], F32, kind="ExternalInput").ap()
    pbT = nc.dram_tensor("pbT", [D, T], F32, kind="ExternalInput").ap()
    gl = nc.dram_tensor("gl", [128, 16], F32, kind="ExternalInput").ap()
    oT = nc.dram_tensor("oT", [D, T], F32, kind="ExternalOutput").ap()
    with ExitStack() as st:
        k = K(nc, st)
        pe, act, dve, pool, sp = k.pe, k.act, k.dve, k.pool, k.sp
        cm = Common(k, N)
        cm.setup_eps()
        g = k.sb([128, 16], F32, "g")
        k.dma(act, g[:], gl[:, :], W=[g])
        xa = [k.sb([128, 16, N], F32, f"xa{i}") for i in range(2)]
        xb = k.sb([128, 16, N], F32, "xb")
        ps = k.ps([128, 512], F32, "ps")
        v1 = x1T.rearrange("(kt p) t -> p kt t", p=128)
        va = paT.rearrange("(kt p) t -> p kt t", p=128)
        vb = pbT.rearrange("(kt p) t -> p kt t", p=128)
        vo = oT.rearrange("(kt p) t -> p kt t", p=128)
        for ti in range(T // N):
            t0 = ti * N
            x_ = xa[ti % 2]
            k.dma(sp, x_[:], v1[:, :, t0:t0 + N], W=[x_])
            k.dma(sp, xb[:], va[:, :, t0:t0 + N], W=[xb])
            k.op(dve, lambda v: v.tensor_tensor(out=x_[:], in0=x_[:], in1=xb[:], op=ALU.add), R=[xb], W=[x_])
            k.dma(sp, xb[:], vb[:, :, t0:t0 + N], W=[xb])
            k.op(dve, lambda v: v.tensor_tensor(out=x_[:], in0=x_[:], in1=xb[:], op=ALU.add), R=[xb], W=[x_])
            cm.rmsnorm(x_, g, ps, xb)
            k.dma(sp, vo[:, :, t0:t0 + N], xb[:], R=[xb])
        k.finish()
    return nc


def barrier(k):
    engs = [k.pe, k.act, k.dve, k.pool, k.sp]
    toks = [(e.sem, e.n) for e in engs if e.n > 0]
    for q in (k.sp, k.act, k.pool):
        for slot in range(NDMA):
            cnt = (q.dn - slot + NDMA - 1) // NDMA if q.dn > slot else 0
            if cnt > 0:
                toks.append((q.dsem[slot], 16 * cnt))
    for e in engs:
        for t in toks:
            if t[0] is e.sem:
                continue
            e.wait(t)


def build_s5(S=4096, N=512, CH=1024):
    from contextlib import ExitStack
    nc = bass.Bass("TRN2", target_bir_lowering=False)
    D = 2048
    NPT = 16
    NYT = 4
    xT = nc.dram_tensor("xT", [D, S], F32, kind="ExternalInput").ap()
    mngl = nc.dram_tensor("mngl", [128, 16], F32, kind="ExternalInput").ap()
    w_u = nc.dram_tensor("w_u", [D, 512], F32, kind="ExternalInput").ap()
    are_l = nc.dram_tensor("are_l", [128, 2 * NPT], F32, kind="ExternalInput").ap()
    aim_l = nc.dram_tensor("aim_l", [128, 2 * NPT], F32, kind="ExternalInput").ap()
    ldt_l = nc.dram_tensor("ldt_l", [128, 2 * NPT], F32, kind="ExternalInput").ap()
    bre_pad = nc.dram_tensor("bre_pad", [128, 2 * NPT, 128], F32, kind="ExternalInput").ap()
    bim_pad = nc.dram_tensor("bim_pad", [128, 2 * NPT, 128], F32, kind="ExternalInput").ap()
    cre_pad = nc.dram_tensor("cre_pad", [128, 2 * NPT, 128], F32, kind="ExternalInput").ap()
    cim_pad = nc.dram_tensor("cim_pad", [128, 2 * NPT, 128], F32, kind="ExternalInput").ap()
    d_pad = nc.dram_tensor("d_pad", [128, NYT, 128], F32, kind="ExternalInput").ap()
    gsT = nc.dram_tensor("gsT", [512, S], F32, kind="ExternalOutput").ap()
    NCK = S // CH
    TWO_PI = 6.283185
    with ExitStack() as st:
        k = K(nc, st)
        pe, act, dve, pool, sp = k.pe, k.act, k.dve, k.pool, k.sp
        uT = k.sb([128, NYT, S], BF16, "uT")
        halfpi = k.sb([128, 1], F32, "halfpi")
        k.op(pool, lambda g: g.memset(halfpi[:], 1.5707963), W=[halfpi])
        pss = [k.ps([128, 1024], F32, f"ps{i}") for i in range(4)]
        with ExitStack() as ph1:
            k.stack = ph1
            cm = Common(k, N)
            cm.setup_eps()
            mng = k.sb([128, 16], F32, "mng")
            k.dma(act, mng[:], mngl[:, :], W=[mng])
            wu = k.sb([128, 16, 512], BF16, "wu")
            cm.load_w(wu, w_u, 512, 16)
            xt = [k.sb([128, 16, N], F32, f"xt{i}") for i in range(2)]
            hT = k.sb([128, 16, N], BF16, "hT")
            xv = xT.rearrange("(kt p) t -> p kt t", p=128)
            for ti in range(S // N):
                t0 = ti * N
                x_ = xt[ti % 2]
                k.dma(sp, x_[:], xv[:, :, t0:t0 + N], W=[x_])
                cm.rmsnorm(x_, mng, pss[3], hT)
                for m in range(NYT):
                    p_ = pss[m % 3]
                    for kt in range(16):
                        k.op(pe, lambda t: t.matmul(p_[:, 0:N], lhsT=wu[:, kt, m * 128:(m + 1) * 128], rhs=hT[:, kt, :], start=(kt == 0), stop=(kt == 15)),
                             R=[wu, hT], W=[p_])
                    k.op(act, lambda a: a.copy(out=uT[:, m, t0:t0 + N], in_=p_[:, 0:N]), R=[p_], W=[uT])
            barrier(k)
        k.stack = st
        NP = 2 * NPT
        prm = {}
        for nm in ("are", "aim", "ldt", "dt", "mag", "thn", "thf", "af", "sn", "cs", "lre", "lim", "den", "nr", "t1", "t2", "cfr", "cfi", "ncfi"):
            prm[nm] = k.sb([128, NP], F32, "prm_" + nm)
        kI = k.sb([128, NP], I32, "prm_kI")
        k.dma(act, prm["are"][:], are_l[:, :], W=[prm["are"]])
        k.dma(act, prm["aim"][:], aim_l[:, :], W=[prm["aim"]])
        k.dma(act, prm["ldt"][:], ldt_l[:, :], W=[prm["ldt"]])

        def tt(o, a, b, op, eng=None):
            k.op(dve, lambda v: v.tensor_tensor(out=prm[o][:], in0=prm[a][:], in1=prm[b][:], op=op), R=[prm[a], prm[b]], W=[prm[o]])

        k.op(act, lambda a: a.activation(out=prm["dt"][:], in_=prm["ldt"][:], func=AF.Exp), R=[prm["ldt"]], W=[prm["dt"]])
        tt("t1", "are", "dt", ALU.mult)
        k.op(act, lambda a: a.activation(out=prm["mag"][:], in_=prm["t1"][:], func=AF.Exp), R=[prm["t1"]], W=[prm["mag"]])
        tt("t2", "aim", "dt", ALU.mult)
        k.op(dve, lambda v: v.tensor_scalar(out=prm["thn"][:], in0=prm["t2"][:], scalar1=1.0 / 6.283185307179586, scalar2=None, op0=ALU.mult), R=[prm["t2"]], W=[prm["thn"]])
        k.op(dve, lambda v: v.tensor_copy(out=kI[:], in_=prm["thn"][:]), R=[prm["thn"]], W=[kI])
        k.op(dve, lambda v: v.tensor_tensor(out=prm["thf"][:], in0=prm["thn"][:], in1=kI[:], op=ALU.subtract), R=[prm["thn"], kI], W=[prm["thf"]])
        k.op(act, lambda a: a.activation(out=prm["af"][:], in_=prm["thf"][:], func=AF.Abs), R=[prm["thf"]], W=[prm["af"]])
        k.op(act, lambda a: a.activation(out=prm["sn"][:], in_=prm["thf"][:], func=AF.Sin, scale=TWO_PI), R=[prm["thf"]], W=[prm["sn"]])
        k.op(act, lambda a: a.activation(out=prm["cs"][:], in_=prm["af"][:], func=AF.Sin, scale=-TWO_PI, bias=halfpi[:]), R=[prm["af"], halfpi], W=[prm["cs"]])
        tt("lre", "mag", "cs", ALU.mult)
        tt("lim", "mag", "sn", ALU.mult)
        tt("t1", "are", "are", ALU.mult)
        tt("t2", "aim", "aim", ALU.mult)
        tt("den", "t1", "t2", ALU.add)
        k.op(dve, lambda v: v.reciprocal(out=prm["den"][:], in_=prm["den"][:]), R=[], W=[prm["den"]])
        k.op(dve, lambda v: v.tensor_scalar(out=prm["nr"][:], in0=prm["lre"][:], scalar1=-1.0, scalar2=None, op0=ALU.add), R=[prm["lre"]], W=[prm["nr"]])
        tt("t1", "nr", "are", ALU.mult)
        tt("t2", "lim", "aim", ALU.mult)
        tt("t1", "t1", "t2", ALU.add)
        tt("cfr", "t1", "den", ALU.mult)
        tt("t1", "lim", "are", ALU.mult)
        tt("t2", "nr", "aim", ALU.mult)
        tt("t1", "t1", "t2", ALU.subtract)
        tt("cfi", "t1", "den", ALU.mult)
        k.op(dve, lambda v: v.tensor_scalar(out=prm["ncfi"][:], in0=prm["cfi"][:], scalar1=-1.0, scalar2=None, op0=ALU.mult), R=[prm["cfi"]], W=[prm["ncfi"]])
        lB = [k.sb([128, NP, 128], BF16, f"lB{i}") for i in range(2)]
        lC = [k.sb([128, NP, 128], BF16, f"lC{i}") for i in range(2)]
        lD = k.sb([128, NYT, 128], BF16, "lD")
        with ExitStack() as ph2:
            k.stack = ph2
            sga = k.sb([128, NP, 128], F32, "sga")
            sgb = k.sb([128, NP, 128], F32, "sgb")
            sgc = k.sb([128, NP, 128], F32, "sgc")
            sgd = k.sb([128, NYT, 128], F32, "sgd")
            k.dma(sp, sga[:], bre_pad[:, :, :], W=[sga])
            k.op(dve, lambda v: v.tensor_copy(out=lB[0][:], in_=sga[:]), R=[sga], W=[lB[0]])
            k.dma(sp, sgb[:], bim_pad[:, :, :], W=[sgb])
            k.op(dve, lambda v: v.tensor_copy(out=lB[1][:], in_=sgb[:]), R=[sgb], W=[lB[1]])
            k.dma(sp, sgd[:], d_pad[:, :, :], W=[sgd])
            k.op(dve, lambda v: v.tensor_copy(out=lD[:], in_=sgd[:]), R=[sgd], W=[lD])
            k.dma(sp, sga[:], cre_pad[:, :, :], W=[sga])
            k.dma(sp, sgb[:], cim_pad[:, :, :], W=[sgb])
            bc = lambda nm: prm[nm][:].unsqueeze(2).to_broadcast([128, NP, 128])
            k.op(dve, lambda v: v.tensor_tensor(out=sgc[:], in0=sga[:], in1=bc("cfr"), op=ALU.mult), R=[sga, prm["cfr"]], W=[sgc])
            k.op(dve, lambda v: v.tensor_tensor(out=sga[:], in0=sga[:], in1=bc("ncfi"), op=ALU.mult), R=[prm["ncfi"]], W=[sga])
            tmpc = k.sb([128, NP, 128], F32, "tmpc")
            k.op(dve, lambda v: v.tensor_tensor(out=tmpc[:], in0=sgb[:], in1=bc("ncfi"), op=ALU.mult), R=[sgb, prm["ncfi"]], W=[tmpc])
            k.op(dve, lambda v: v.tensor_tensor(out=lC[0][:], in0=sgc[:], in1=tmpc[:], op=ALU.add), R=[sgc, tmpc], W=[lC[0]])
            k.op(dve, lambda v: v.tensor_tensor(out=tmpc[:], in0=sgb[:], in1=bc("cfr"), op=ALU.mult), R=[sgb, prm["cfr"]], W=[tmpc])
            k.op(dve, lambda v: v.tensor_tensor(out=lC[1][:], in0=sga[:], in1=tmpc[:], op=ALU.subtract), R=[sga, tmpc], W=[lC[1]])
            barrier(k)
        k.stack = st
        iota_i = k.sb([128, CH], I32, "iota_i")
        iota_f = k.sb([128, CH], F32, "iota_f")
        k.op(pool, lambda g: g.iota(out=iota_i[:], pattern=[[1, CH]], base=0, channel_multiplier=0), W=[iota_i])
        k.op(dve, lambda v: v.tensor_copy(out=iota_f[:], in_=iota_i[:]), R=[iota_i], W=[iota_f])
        W_ = lambda nm, dt=F32, n=2: [k.sb([128, CH], dt, f"{nm}{i}") for i in range(n)]
        kq, fq, aq, cq, sq_ = W_("kq", I32, 1), W_("fq", F32, 1), W_("aq", F32, 1), W_("cq"), W_("sq")
        t1, t2 = W_("t1_", F32, 1), W_("t2_", F32, 1)
        xr, xi, gr, gi_ = W_("xr"), W_("xi"), W_("gr"), W_("gi")
        q1, q2 = W_("q1", F32, 1), W_("q2", F32, 1)
        hr, hi = W_("hr", BF16), W_("hi", BF16)
        carry = [k.sb([128, 2], F32, f"carry{i}") for i in range(2)]
        yacc = k.sb([128, S], F32, "yacc")
        gout = k.sb([128, S], F32, "gout")
        it = 0
        for yt in range(NYT):
            for c in range(NCK):
                p_ = pss[2]
                for hh in range(CH // 512):
                    k.op(pe, lambda t: t.matmul(p_[:, hh * 512:(hh + 1) * 512], lhsT=lD[:, yt, :], rhs=uT[:, yt, c * CH + hh * 512:c * CH + (hh + 1) * 512], start=True, stop=True),
                         R=[lD, uT], W=[p_])
                k.op(act, lambda a: a.copy(out=yacc[:, c * CH:(c + 1) * CH], in_=p_[:, 0:CH]), R=[p_], W=[yacc])
            for pt in range(4 * yt, 4 * yt + 4):
                for dr in range(2):
                    col = dr * NPT + pt
                    for ci in range(NCK):
                        c = ci if dr == 0 else NCK - 1 - ci
                        b = it % 2
                        it += 1
                        tsl = slice(c * CH, (c + 1) * CH)
                        rev = (lambda ap: ap) if dr == 0 else (lambda ap: ap[:, ::-1])
                        for (pp, lb) in ((pss[0], lB[0]), (pss[1], lB[1])):
                            for hh in range(CH // 512):
                                k.op(pe, lambda t: t.matmul(pp[:, hh * 512:(hh + 1) * 512], lhsT=lb[:, col, :], rhs=uT[:, yt, c * CH + hh * 512:c * CH + (hh + 1) * 512], start=True, stop=True),
                                     R=[lb, uT], W=[pp])
                        k.op(dve, lambda v: v.tensor_scalar(out=kq[0][:], in0=iota_f[:], scalar1=float(ci * CH), scalar2=prm["thf"][:, col:col + 1], op0=ALU.add, op1=ALU.mult),
                             R=[iota_f, prm["thf"]], W=[kq[0]])
                        k.op(dve, lambda v: v.tensor_scalar(out=fq[0][:], in0=iota_f[:], scalar1=float(ci * CH), scalar2=prm["thf"][:, col:col + 1], op0=ALU.add, op1=ALU.mult),
                             R=[iota_f, prm["thf"]], W=[fq[0]])
                        k.op(dve, lambda v: v.tensor_tensor(out=fq[0][:], in0=fq[0][:], in1=kq[0][:], op=ALU.subtract), R=[kq[0]], W=[fq[0]])
                        k.op(act, lambda a: a.activation(out=aq[0][:], in_=fq[0][:], func=AF.Abs), R=[fq[0]], W=[aq[0]])
                        k.op(act, lambda a: a.activation(out=sq_[b][:], in_=fq[0][:], func=AF.Sin, scale=TWO_PI), R=[fq[0]], W=[sq_[b]])
                        k.op(act, lambda a: a.activation(out=cq[b][:], in_=aq[0][:], func=AF.Sin, scale=-TWO_PI, bias=halfpi[:]), R=[aq[0], halfpi], W=[cq[b]])
                        vr, vi = rev(pss[0][:, 0:CH]), rev(pss[1][:, 0:CH])
                        k.op(dve, lambda v: v.tensor_tensor(out=t1[0][:], in0=vr, in1=cq[b][:], op=ALU.mult), R=[pss[0], cq[b]], W=[t1[0]])
                        k.op(dve, lambda v: v.tensor_tensor(out=t2[0][:], in0=vi, in1=sq_[b][:], op=ALU.mult), R=[pss[1], sq_[b]], W=[t2[0]])
                        k.op(pool, lambda g: g.tensor_tensor(out=xr[b][:], in0=t1[0][:], in1=t2[0][:], op=ALU.add), R=[t1[0], t2[0]], W=[xr[b]])
                        k.op(dve, lambda v: v.tensor_tensor(out=t1[0][:], in0=vi, in1=cq[b][:], op=ALU.mult), R=[pss[1], cq[b]], W=[t1[0]])
                        k.op(dve, lambda v: v.tensor_tensor(out=t2[0][:], in0=vr, in1=sq_[b][:], op=ALU.mult), R=[pss[0], sq_[b]], W=[t2[0]])
                        k.op(pool, lambda g: g.tensor_tensor(out=xi[b][:], in0=t1[0][:], in1=t2[0][:], op=ALU.subtract), R=[t1[0], t2[0]], W=[xi[b]])
                        rb = prm["mag"][:, col:col + 1].to_broadcast([128, CH])
                        cprev = carry[(ci + 1) % 2]
                        ccur = carry[ci % 2]
                        ini_r = 0.0 if ci == 0 else cprev[:, 0:1]
                        ini_i = 0.0 if ci == 0 else cprev[:, 1:2]
                        k.op(dve, lambda v: v.tensor_tensor_scan(out=gr[b][:], data0=rb, data1=xr[b][:], initial=ini_r, op0=ALU.mult, op1=ALU.add),
                             R=[prm["mag"], xr[b], cprev], W=[gr[b]])
                        k.op(dve, lambda v: v.tensor_tensor_scan(out=gi_[b][:], data0=rb, data1=xi[b][:], initial=ini_i, op0=ALU.mult, op1=ALU.add),
                             R=[prm["mag"], xi[b], cprev], W=[gi_[b]])
                        k.op(act, lambda a: a.copy(out=ccur[:, 0:1], in_=gr[b][:, CH - 1:CH]), R=[gr[b]], W=[ccur])
                        k.op(act, lambda a: a.copy(out=ccur[:, 1:2], in_=gi_[b][:, CH - 1:CH]), R=[gi_[b]], W=[ccur])
                        k.op(pool, lambda g: g.tensor_tensor(out=q1[0][:], in0=gr[b][:], in1=cq[b][:], op=ALU.mult), R=[gr[b], cq[b]], W=[q1[0]])
                        k.op(pool, lambda g: g.tensor_tensor(out=q2[0][:], in0=gi_[b][:], in1=sq_[b][:], op=ALU.mult), R=[gi_[b], sq_[b]], W=[q2[0]])
                        k.op(pool, lambda g: g.tensor_tensor(out=rev(hr[b][:]), in0=q1[0][:], in1=q2[0][:], op=ALU.subtract), R=[q1[0], q2[0]], W=[hr[b]])
                        k.op(pool, lambda g: g.tensor_tensor(out=q1[0][:], in0=gr[b][:], in1=sq_[b][:], op=ALU.mult), R=[gr[b], sq_[b]], W=[q1[0]])
                        k.op(pool, lambda g: g.tensor_tensor(out=q2[0][:], in0=gi_[b][:], in1=cq[b][:], op=ALU.mult), R=[gi_[b], cq[b]], W=[q2[0]])
                        k.op(pool, lambda g: g.tensor_tensor(out=rev(hi[b][:]), in0=q1[0][:], in1=q2[0][:], op=ALU.add), R=[q1[0], q2[0]], W=[hi[b]])
                        p_ = pss[2]
                        for hh in range(CH // 512):
                            k.op(pe, lambda t: t.matmul(p_[:, hh * 512:(hh + 1) * 512], lhsT=lC[0][:, col, :], rhs=hr[b][:, hh * 512:(hh + 1) * 512], start=True, stop=False),
                                 R=[lC[0], hr[b]], W=[p_])
                            k.op(pe, lambda t: t.matmul(p_[:, hh * 512:(hh + 1) * 512], lhsT=lC[1][:, col, :], rhs=hi[b][:, hh * 512:(hh + 1) * 512], start=False, stop=True),
                                 R=[lC[1], hi[b]], W=[p_])
                        k.op(dve, lambda v: v.tensor_tensor(out=yacc[:, tsl], in0=yacc[:, tsl], in1=p_[:, 0:CH], op=ALU.add), R=[p_], W=[yacc])
            k.op(act, lambda a: a.activation(out=gout[:], in_=yacc[:], func=AF.Gelu_apprx_tanh), R=[yacc], W=[gout])
            k.dma(sp, gsT[yt * 128:(yt + 1) * 128, :], gout[:], R=[gout])
        k.finish()
    return nc


def s5_host_layout(inp, ch):
    G0 = ch * 32
    NPT = 16

    def per_chan(a):
        a = a[:, G0:G0 + 32, :].reshape(2, NPT, 2, 64)
        return np.ascontiguousarray(a.transpose(2, 3, 0, 1).reshape(128, 2 * NPT))
    are = per_chan(inp["ssm_a_re"][0])
    aim = per_chan(inp["ssm_a_im"][0])
    ldt = np.broadcast_to(inp["ssm_log_dt"][0][:, :, None], (2, 64, 64))
    ldt = per_chan(ldt)
    bre = np.zeros((128, 2 * NPT, 128), np.float32)
    bim = np.zeros((128, 2 * NPT, 128), np.float32)
    cre = np.zeros((128, 2 * NPT, 128), np.float32)
    cim = np.zeros((128, 2 * NPT, 128), np.float32)
    for dr in range(2):
        for pt in range(NPT):
            for gi in range(2):
                g = G0 + 2 * pt + gi
                r0 = (pt % 4) * 32 + gi * 16
                col = dr * NPT + pt
                bre[r0:r0 + 16, col, gi * 64:(gi + 1) * 64] = inp["ssm_b_re"][0, dr, g].T
                bim[r0:r0 + 16, col, gi * 64:(gi + 1) * 64] = inp["ssm_b_im"][0, dr, g].T
                cre[gi * 64:(gi + 1) * 64, col, r0:r0 + 16] = inp["ssm_c_re"][0, dr, g].T
                cim[gi * 64:(gi + 1) * 64, col, r0:r0 + 16] = inp["ssm_c_im"][0, dr, g].T
    dpad = np.zeros((128, 4, 128), np.float32)
    dv = inp["ssm_d"][0][ch * 512:(ch + 1) * 512].reshape(4, 128)
    for yt in range(4):
        dpad[np.arange(128), yt, np.arange(128)] = dv[yt]
    return dict(are_l=are, aim_l=aim, ldt_l=ldt, bre_pad=bre, bim_pad=bim, cre_pad=cre, cim_pad=cim, d_pad=dpad)


def build_att(TO=2048, N=256):
    from contextlib import ExitStack
    nc = bass.Bass("TRN2", target_bir_lowering=False)
    D = 2048
    TE = TO + 256
    NBLK = TO // 128
    xT = nc.dram_tensor("xT", [D, TE], F32, kind="ExternalInput").ap()
    mngl = nc.dram_tensor("mngl", [128, 16], F32, kind="ExternalInput").ap()
    w_qkv = nc.dram_tensor("w_qkv", [D, 1536], F32, kind="ExternalInput").ap()
    biasmat = nc.dram_tensor("biasmat", [128, 8, 384], F32, kind="ExternalInput").ap()
    maskmat = nc.dram_tensor("maskmat", [128, 384], F32, kind="ExternalInput").ap()
    edge = nc.dram_tensor("edge", [128, 2], F32, kind="ExternalInput").ap()
    sinkb = nc.dram_tensor("sinkb", [128, 8], F32, kind="ExternalInput").ap()
    attnT = nc.dram_tensor("attnT", [1024, TO], BF16, kind="ExternalOutput").ap()
    SCALE = 128 ** -0.5
    with ExitStack() as st:
        k = K(nc, st)
        pe, act, dve, pool, sp = k.pe, k.act, k.dve, k.pool, k.sp
        cm = Common(k, N)
        cm.setup_eps()
        mng = k.sb([128, 16], F32, "mng")
        k.dma(act, mng[:], mngl[:, :], W=[mng])
        bm = k.sb([128, 8, 384], F32, "bm")
        mm = k.sb([128, 384], F32, "mm")
        edg = k.sb([128, 2], F32, "edg")
        snk = k.sb([128, 8], F32, "snk")
        k.dma(act, bm[:], biasmat[:, :, :], W=[bm])
        k.dma(act, mm[:], maskmat[:, :], W=[mm])
        k.dma(act, edg[:], edge[:, :], W=[edg])
        k.dma(act, snk[:], sinkb[:, :], W=[snk])
        k.op(dve, lambda v: v.tensor_tensor(out=bm[:], in0=bm[:], in1=mm[:].unsqueeze(1).to_broadcast([128, 8, 384]), op=ALU.add), R=[mm], W=[bm])
        wq = k.sb([128, 16, 1024], BF16, "wq")
        wk = k.sb([128, 16, 256], BF16, "wk")
        wv = k.sb([128, 16, 256], BF16, "wv")
        cm.load_w(wq, w_qkv, 1024, 16, col0=0)
        cm.load_w(wk, w_qkv, 256, 16, col0=1024)
        cm.load_w(wv, w_qkv, 256, 16, col0=1280)
        qT = k.sb([128, 8, TO], BF16, "qT")
        kT = k.sb([128, 2, TE], BF16, "kT")
        vtok = k.sb([128, TE // 128, 256], BF16, "vtok")
        xt = k.sb([128, 16, N], F32, "xt")
        hT = k.sb([128, 16, N], BF16, "hT")
        pss = [k.ps([128, 512], F32, f"ps{i}") for i in range(8)]
        npz = [0]

        def nps():
            npz[0] += 1
            return pss[npz[0] % 3]
        xv = xT.rearrange("(kt p) t -> p kt t", p=128)
        for ti in range(TE // N):
            t0 = ti * N
            k.dma(sp, xt[:], xv[:, :, t0:t0 + N], W=[xt])
            cm.rmsnorm(xt, mng, pss[3], hT)
            lo = max(t0, 128)
            hi = min(t0 + N, 128 + TO)
            for m in range(8):
                p_ = nps()
                for kt in range(16):
                    k.op(pe, lambda t: t.matmul(p_[:, 0:N], lhsT=wq[:, kt, m * 128:(m + 1) * 128], rhs=hT[:, kt, :], start=(kt == 0), stop=(kt == 15)),
                         R=[wq, hT], W=[p_])
                if hi > lo:
                    k.op(act, lambda a: a.copy(out=qT[:, m, lo - 128:hi - 128], in_=p_[:, lo - t0:hi - t0]), R=[p_], W=[qT])
            for m in range(2):
                p_ = nps()
                for kt in range(16):
                    k.op(pe, lambda t: t.matmul(p_[:, 0:N], lhsT=wk[:, kt, m * 128:(m + 1) * 128], rhs=hT[:, kt, :], start=(kt == 0), stop=(kt == 15)),
                         R=[wk, hT], W=[p_])
                k.op(act, lambda a: a.copy(out=kT[:, m, t0:t0 + N], in_=p_[:, 0:N]), R=[p_], W=[kT])
            for c in range(N // 128):
                p_ = nps()
                for kt in range(16):
                    k.op(pe, lambda t: t.matmul(p_[:, 0:256], lhsT=hT[:, kt, c * 128:(c + 1) * 128], rhs=wv[:, kt, :], start=(kt == 0), stop=(kt == 15)),
                         R=[wv, hT], W=[p_])
                k.op(act, lambda a: a.copy(out=vtok[:, t0 // 128 + c, :], in_=p_[:, 0:256]), R=[p_], W=[vtok])
        s_sb = [k.sb([128, 384], F32, f"s_sb{i}") for i in range(2)]
        p_sb = [k.sb([128, 384], F32, f"p_sb{i}") for i in range(2)]
        pT = [k.sb([128, 384], BF16, f"pT{i}") for i in range(2)]
        sml = [k.sb([128, 8], F32, f"sml{i}") for i in range(2)]
        ob = [k.sb([128, 8, 512], BF16, f"ob{i}") for i in range(2)]
        av = attnT.rearrange("(h p) t -> p h t", p=128)
        it = 0
        for blk in range(NBLK):
            o_ = ob[(blk // 4) % 2]
            for hd in range(8):
                kv = hd // 4
                b = it % 2
                it += 1
                ps_s, ps_t, ps_o = pss[4 + (it % 2) * 0], pss[5], pss[6 + (it % 2)]
                ps_s = pss[4] if it % 2 == 0 else pss[7]
                ps_o = pss[6]
                k.op(pe, lambda t: t.matmul(ps_s[:, 0:384], lhsT=qT[:, hd, blk * 128:(blk + 1) * 128], rhs=kT[:, kv, blk * 128:blk * 128 + 384], start=True, stop=True),
                     R=[qT, kT], W=[ps_s])
                s_, p_, sm = s_sb[b], p_sb[b], sml[b]
                k.op(dve, lambda v: v.scalar_tensor_tensor(out=s_[:], in0=ps_s[:, 0:384], scalar=SCALE, in1=bm[:, hd, :], op0=ALU.mult, op1=ALU.add),
                     R=[ps_s, bm], W=[s_])
                if blk == 0:
                    k.op(dve, lambda v: v.tensor_scalar(out=s_[:, 0:128], in0=s_[:, 0:128], scalar1=edg[:, 0:1], scalar2=None, op0=ALU.add), R=[edg], W=[s_])
                if blk == NBLK - 1:
                    k.op(dve, lambda v: v.tensor_scalar(out=s_[:, 256:384], in0=s_[:, 256:384], scalar1=edg[:, 1:2], scalar2=None, op0=ALU.add), R=[edg], W=[s_])
                k.op(dve, lambda v: v.tensor_reduce(out=sm[:, 0:1], in_=s_[:], axis=AX.X, op=ALU.max), R=[s_], W=[sm])
                k.op(dve, lambda v: v.tensor_tensor(out=sm[:, 1:2], in0=sm[:, 0:1], in1=snk[:, hd:hd + 1], op=ALU.max), R=[snk], W=[sm])
                k.op(dve, lambda v: v.tensor_scalar(out=sm[:, 2:3], in0=sm[:, 1:2], scalar1=-1.0, scalar2=None, op0=ALU.mult), R=[], W=[sm])
                k.op(act, lambda a: a.activation(out=p_[:], in_=s_[:], func=AF.Exp, bias=sm[:, 2:3], accum_out=sm[:, 3:4]), R=[s_, sm], W=[p_, sm])
                k.op(act, lambda a: a.activation(out=sm[:, 4:5], in_=snk[:, hd:hd + 1], func=AF.Exp, bias=sm[:, 2:3]), R=[snk], W=[sm])
                k.op(dve, lambda v: v.tensor_tensor(out=sm[:, 5:6], in0=sm[:, 3:4], in1=sm[:, 4:5], op=ALU.add), R=[], W=[sm])
                k.op(dve, lambda v: v.reciprocal(out=sm[:, 6:7], in_=sm[:, 5:6]), R=[], W=[sm])
                k.op(dve, lambda v: v.tensor_scalar(out=p_[:], in0=p_[:], scalar1=sm[:, 6:7], scalar2=None, op0=ALU.mult), R=[sm], W=[p_])
                for j in range(3):
                    k.op(pe, lambda t: t.transpose(out=ps_t[:, j * 128:(j + 1) * 128], in_=p_[:, j * 128:(j + 1) * 128], identity=cm.identf[:]),
                         R=[p_, cm.identf], W=[ps_t])
                k.op(act, lambda a: a.copy(out=pT[b][:], in_=ps_t[:, 0:384]), R=[ps_t], W=[pT[b]])
                for j in range(3):
                    k.op(pe, lambda t: t.matmul(ps_o[:, 0:128], lhsT=vtok[:, blk + j, kv * 128:(kv + 1) * 128], rhs=pT[b][:, j * 128:(j + 1) * 128], start=(j == 0), stop=(j == 2)),
                         R=[vtok, pT[b]], W=[ps_o])
                k.op(act, lambda a: a.copy(out=o_[:, hd, (blk % 4) * 128:(blk % 4 + 1) * 128], in_=ps_o[:, 0:128]), R=[ps_o], W=[o_])
            if blk % 4 == 3:
                k.dma(sp, av[:, :, (blk // 4) * 512:(blk // 4 + 1) * 512], o_[:], R=[o_])
        k.finish()
    return nc


T5_ROUND_NEAREST = False


def t5_bucket_np(rel):
    import math
    nb, max_exact = 16, 8
    base = np.where(rel > 0, nb, 0)
    n = np.abs(rel)
    nf = np.maximum(n, 1).astype(np.float32)
    val = np.log(nf / np.float32(max_exact)) / np.float32(math.log(128 / max_exact)) * np.float32(nb - max_exact)
    large = max_exact + (np.rint(val).astype(np.int32) if T5_ROUND_NEAREST else val.astype(np.int32))
    large = np.minimum(large, nb - 1)
    return base + np.where(n < max_exact, n, large)


def att_host_consts(rel_bias, attn_sink):
    q = np.arange(128)[:, None]
    c = np.arange(384)[None, :]
    rel = (c - 128 - q).astype(np.int32)
    inband = np.abs(rel) <= 128
    bidx = np.where(inband, t5_bucket_np(rel), 32)
    rb = np.concatenate([rel_bias, np.zeros((1, 8), np.float32)], 0)
    biasmat = np.ascontiguousarray(rb[bidx].transpose(0, 2, 1))
    maskmat = np.where(inband, 0.0, -1e30).astype(np.float32)
    sinkb = np.ascontiguousarray(np.broadcast_to(attn_sink.reshape(1, 8), (128, 8))).astype(np.float32)
    return biasmat.astype(np.float32), maskmat, sinkb


_PROGS = {}


def _prog(name, fn):
    if name not in _PROGS:
        _PROGS[name] = fn()
    return _PROGS[name]


def _run(nc, in_maps):
    res = run_bass_kernel_spmd(nc, in_maps, core_ids=list(range(8)))
    return res.results


def _lay(v, kt):
    return np.ascontiguousarray(np.asarray(v, np.float32).reshape(kt, 128).T)


def _bc(v):
    v = np.asarray(v, np.float32)
    return np.ascontiguousarray(np.broadcast_to(v, (128,) + v.shape))


def kernel(**inp):
    inp = {k_: np.asarray(v) for k_, v in inp.items()}
    x = inp["x"].astype(np.float32, copy=False)
    B, S, D = x.shape
    H = S // 2
    cores = [(b, hf) for b in range(B) for hf in range(2)]
    xT = [np.ascontiguousarray(x[b].T) for b in range(B)]

    bmat, mmat, sinkb = att_host_consts(inp["rel_bias"].astype(np.float32), inp["attn_sink"][0].astype(np.float32))
    mng0 = _lay(inp["mix_norm"][0], 16)
    w_in0 = inp["even_w_in"][0]
    w_qkv = np.ascontiguousarray(w_in0[:, :1536])
    maps = []
    for (b, hf) in cores:
        xe = np.zeros((D, H + 256), np.float32)
        lo = hf * H - 128
        a0, a1 = max(lo, 0), min(lo + H + 256, S)
        xe[:, a0 - lo:a1 - lo] = xT[b][:, a0:a1]
        edge = np.zeros((128, 2), np.float32)
        edge[:, 0 if hf == 0 else 1] = -1e30
        maps.append(dict(xT=xe, mngl=mng0, w_qkv=w_qkv, biasmat=bmat, maskmat=mmat, edge=edge, sinkb=sinkb))
    r_att = _run(_prog("att", build_att), maps)

    maps = []
    for (b, ch) in cores:
        m = dict(xT=xT[b], mngl=mng0, w_u=np.ascontiguousarray(w_in0[:, 1536 + ch * 512:1536 + (ch + 1) * 512]))
        m.update(s5_host_layout(inp, ch))
        maps.append(m)
    r_s5 = _run(_prog("s5", build_s5), maps)

    fng0 = _lay(inp["ffn_norm"][0], 16)
    glu_bl = _lay(inp["glu_b"][0], 8)
    maps = []
    for i, (b, hf) in enumerate(cores):
        gs = np.concatenate([r_s5[2 * b]["gsT"], r_s5[2 * b + 1]["gsT"]], axis=0)
        maps.append(dict(xT=np.ascontiguousarray(xT[b][:, hf * H:(hf + 1) * H]), attnT=r_att[i]["attnT"],
                         gsT=np.ascontiguousarray(gs[:, hf * H:(hf + 1) * H]), glu_w=inp["glu_w"][0], glu_bl=glu_bl,
                         w_out=inp["even_w_out"][0], fngl=fng0, router=inp["router"][0]))
    r_c = _run(_prog("c", build_c), maps)

    def moe_layer(r_prev, layer):
        maps = []
        for (b, eh) in cores:
            affT = np.concatenate([r_prev[2 * b]["affT"], r_prev[2 * b + 1]["affT"]], axis=0)
            h = np.concatenate([r_prev[2 * b]["h2T"], r_prev[2 * b + 1]["h2T"]], axis=1)
            maps.append(dict(aff=np.ascontiguousarray(affT.T[eh * 8:(eh + 1) * 8]), h=np.ascontiguousarray(h.T),
                             w1=inp["moe_w1"][layer, eh * 8:(eh + 1) * 8], w3=inp["moe_w3"][layer, eh * 8:(eh + 1) * 8],
                             w2=inp["moe_w2"][layer, eh * 8:(eh + 1) * 8]))
        return _run(_prog("moe", build_moe), maps)

    r_m0 = moe_layer(r_c, 0)

    def partials(r_m, b, hf):
        pa = np.ascontiguousarray(r_m[2 * b]["y"][hf * H:(hf + 1) * H].T)
        pb = np.ascontiguousarray(r_m[2 * b + 1]["y"][hf * H:(hf + 1) * H].T)
        return pa, pb
    maps = []
    for i, (b, hf) in enumerate(cores):
        pa, pb = partials(r_m0, b, hf)
        maps.append(dict(x1T=r_c[i]["x1T"], paT=pa, pbT=pb, mngl=_lay(inp["mix_norm"][1], 16), w_in=inp["odd_w_in"][0],
                         lng_bc=_bc(inp["sgu_ln_g"][0]), lnb_bc=_bc(inp["sgu_ln_b"][0]),
                         sgu_wT=np.ascontiguousarray(inp["sgu_w"][0].transpose(2, 0, 1)), sgu_b_bc=_bc(inp["sgu_b"][0]),
                         w_out=inp["odd_w_out"][0], fngl=_lay(inp["ffn_norm"][1], 16), router=inp["router"][1]))
    r_e = _run(_prog("e", build_e), maps)
    r_m1 = moe_layer(r_e, 1)

    maps = []
    for i, (b, hf) in enumerate(cores):
        pa, pb = partials(r_m1, b, hf)
        maps.append(dict(x1T=r_e[i]["x1T_out"], paT=pa, pbT=pb, gl=_lay(inp["final_norm"], 16)))
    r_g = _run(_prog("g", build_g), maps)
    out = np.empty((B, S, D), np.float32)
    for i, (b, hf) in enumerate(cores):
        out[b, hf * H:(hf + 1) * H] = r_g[i]["oT"].T
    return out
```
